# Optimizing a Trainium2 kernel written in Bass

```python
import jax, jax.numpy as jnp
from jax import lax
import numpy as np

D_MODEL = 1024
BATCH = 2
SEQ = 8192
DEPTH = 4

D_MIX = D_MODEL
GLA_HEADS = 4
GLA_DK = 64
GLA_DV = 128
GLA_QK = GLA_HEADS * GLA_DK
GLA_WIDTH = GLA_HEADS * GLA_DV
GLA_LOWRANK = 16
GLA_TAU = 16.0
GLA_CHUNK = 64
SSD_HEADS = 8
SSD_HEADDIM = 64
SSD_WIDTH = SSD_HEADS * SSD_HEADDIM
SSD_GROUPS = 2
SSD_STATE = 64
SSD_BC = SSD_GROUPS * SSD_STATE
SSD_CONV = 4
SSD_CONV_DIM = SSD_WIDTH + 2 * SSD_BC
SSD_CHUNK = 128
D_FF = 3584
N_EXPERTS = 8
TOP_K = 2
EPS = 1e-6

IN_SIZES = (GLA_QK, GLA_QK, GLA_WIDTH, GLA_WIDTH, GLA_LOWRANK,
            SSD_WIDTH, SSD_WIDTH, SSD_BC, SSD_BC, SSD_HEADS)
IN_SPLITS = [int(s) for s in np.cumsum(IN_SIZES)[:-1]]
D_IN_PROJ = int(sum(IN_SIZES))

kernel_name = "hybrid_gla_ssd_adaln_moe_trunk"


def rmsnorm(x, w):
    x32 = x.astype(jnp.float32)
    r = x32 * lax.rsqrt(jnp.mean(x32 * x32, axis=-1, keepdims=True) + EPS)
    return (r * w.astype(jnp.float32)).astype(x.dtype)


def inter_chunk_states(decay, contrib):
    d = jnp.moveaxis(decay, 1, 0)
    u = jnp.moveaxis(contrib, 1, 0)

    def step(s, inp):
        dn, un = inp
        return dn * s + un, s

    _, prev = lax.scan(step, jnp.zeros_like(u[0]), (d, u))
    return jnp.moveaxis(prev, 0, 1)


def gla_chunked(q, k, v, log_a):
    bsz, seq, nh, dk = q.shape
    dv = v.shape[-1]
    nc = seq // GLA_CHUNK
    shp = lambda t: t.astype(jnp.float32).reshape(bsz, nc, GLA_CHUNK, nh, t.shape[-1])
    q, k, v, log_a = shp(q) * (dk ** -0.5), shp(k), shp(v), shp(log_a)
    b = jnp.cumsum(log_a, axis=2)
    qe = q * jnp.exp(b)
    ke = k * jnp.exp(-b)
    causal = jnp.tril(jnp.ones((GLA_CHUNK, GLA_CHUNK), bool))
    att = jnp.einsum('bnihd,bnjhd->bnhij', qe, ke)
    att = jnp.where(causal, att, 0.0)
    o_intra = jnp.einsum('bnhij,bnjhv->bnihv', att, v)
    b_last = b[:, :, -1]
    k_tail = k * jnp.exp(b_last[:, :, None] - b)
    contrib = jnp.einsum('bnjhd,bnjhv->bnhdv', k_tail, v)
    s_prev = inter_chunk_states(jnp.exp(b_last)[..., None], contrib)
    o_inter = jnp.einsum('bnihd,bnhdv->bnihv', qe, s_prev)
    return (o_intra + o_inter).reshape(bsz, seq, nh, dv)


def ssd_chunked(xh, dt, a, bm, cm):
    bsz, seq, nh, p = xh.shape
    rep = nh // bm.shape[2]
    nc = seq // SSD_CHUNK
    f = lambda t: t.astype(jnp.float32).reshape((bsz, nc, SSD_CHUNK) + t.shape[2:])
    xh, dt = f(xh), f(dt)
    bh = f(jnp.repeat(bm, rep, axis=2))
    ch = f(jnp.repeat(cm, rep, axis=2))
    cum = jnp.cumsum(dt * a, axis=2)
    causal = jnp.tril(jnp.ones((SSD_CHUNK, SSD_CHUNK), bool))[:, :, None]
    seg = cum[:, :, :, None, :] - cum[:, :, None, :, :]
    decay = jnp.exp(jnp.where(causal, seg, -jnp.inf))
    xdt = xh * dt[..., None]
    scores = jnp.einsum('bnihs,bnjhs->bnijh', ch, bh) * decay
    y_intra = jnp.einsum('bnijh,bnjhp->bnihp', scores, xdt)
    cum_last = cum[:, :, -1]
    contrib = jnp.einsum('bnjhs,bnjhp->bnhps',
                         bh * jnp.exp(cum_last[:, :, None] - cum)[..., None], xdt)
    s_prev = inter_chunk_states(jnp.exp(cum_last)[..., None, None], contrib)
    y_inter = jnp.einsum('bnihs,bnhps->bnihp', ch * jnp.exp(cum)[..., None], s_prev)
    return (y_intra + y_inter).reshape(bsz, seq, nh, p)


def causal_depthwise_conv(u, w, b):
    kw, cdim = w.shape
    out = lax.conv_general_dilated(u, w.astype(u.dtype)[:, None, :], window_strides=(1,),
                                   padding=[(kw - 1, 0)],
                                   dimension_numbers=('NWC', 'WIO', 'NWC'),
                                   feature_group_count=cdim)
    return out + b.astype(u.dtype)


def hybrid_mixer(h, w_in, gla_a_w, gla_a_b, gla_norm_w, conv_w, conv_b,
                 dt_bias, a_log, d_skip, ssd_norm_w, w_out):
    bsz, seq, _ = h.shape
    proj = h @ w_in
    q, k, v, g, alr, z, xs, bm, cm, dt = jnp.split(proj, IN_SPLITS, axis=-1)
    log_a = jax.nn.log_sigmoid((alr @ gla_a_w + gla_a_b).astype(jnp.float32)) / GLA_TAU
    o = gla_chunked(q.reshape(bsz, seq, GLA_HEADS, GLA_DK),
                    k.reshape(bsz, seq, GLA_HEADS, GLA_DK),
                    v.reshape(bsz, seq, GLA_HEADS, GLA_DV),
                    log_a.reshape(bsz, seq, GLA_HEADS, GLA_DK))
    o = rmsnorm(o, gla_norm_w) * jax.nn.silu(g.astype(jnp.float32).reshape(bsz, seq, GLA_HEADS, GLA_DV))
    o = o.reshape(bsz, seq, GLA_WIDTH)
    xbc = jax.nn.silu(causal_depthwise_conv(jnp.concatenate([xs, bm, cm], axis=-1), conv_w, conv_b))
    xs, bm, cm = jnp.split(xbc, [SSD_WIDTH, SSD_WIDTH + SSD_BC], axis=-1)
    dt = jax.nn.softplus(dt.astype(jnp.float32) + dt_bias.astype(jnp.float32))
    a = -jnp.exp(a_log.astype(jnp.float32))
    xh = xs.reshape(bsz, seq, SSD_HEADS, SSD_HEADDIM)
    y = ssd_chunked(xh, dt, a,
                    bm.reshape(bsz, seq, SSD_GROUPS, SSD_STATE),
                    cm.reshape(bsz, seq, SSD_GROUPS, SSD_STATE))
    y = y + d_skip.astype(jnp.float32)[:, None] * xh.astype(jnp.float32)
    y = y.reshape(bsz, seq, SSD_WIDTH) * jax.nn.silu(z.astype(jnp.float32))
    y = rmsnorm(y, ssd_norm_w)
    merged = jnp.concatenate([o, y], axis=-1).astype(h.dtype)
    return merged @ w_out


def swiglu(t, w_gate, w_up, w_down):
    return (jax.nn.silu(t @ w_gate) * (t @ w_up)) @ w_down


def moe_swiglu(h, router_w, w_gate, w_up, w_down):
    bsz, seq, d = h.shape
    t = h.reshape(bsz * seq, d)
    logits = (t @ router_w).astype(jnp.float32)
    top_v, top_i = lax.top_k(logits, TOP_K)
    top_p = jax.nn.softmax(top_v, axis=-1)
    gates = jnp.sum(jax.nn.one_hot(top_i, N_EXPERTS, dtype=jnp.float32) * top_p[..., None], axis=1)
    gates = gates.astype(h.dtype)
    out = jnp.zeros_like(t)
    for e in range(N_EXPERTS):
        out = out + gates[:, e:e + 1] * swiglu(t, w_gate[e], w_up[e], w_down[e])
    return out.reshape(bsz, seq, d)


def setup_inputs(seed: int = 0) -> dict:
    key = jax.random.key(seed)
    ks = iter(list(jax.random.split(key, 40)))
    f32 = jnp.float32
    nrm = lambda shape, scale: jax.random.normal(next(ks), shape, f32) * scale
    n_dense = (DEPTH + 1) // 2
    n_moe = DEPTH // 2
    d = D_MODEL
    x = nrm((BATCH, SEQ, d), 1.0)
    c = nrm((BATCH, d), 1.0)
    ada_w = nrm((DEPTH, d, 6 * d), 0.5 * d ** -0.5)
    ada_b = nrm((DEPTH, 6 * d), 0.02)
    norm1_w = 1.0 + nrm((DEPTH, d), 0.02)
    w_in = nrm((DEPTH, d, D_IN_PROJ), d ** -0.5)
    gla_a_w = nrm((DEPTH, GLA_LOWRANK, GLA_QK), GLA_LOWRANK ** -0.5)
    gla_a_b = nrm((DEPTH, GLA_QK), 0.1)
    gla_norm_w = 1.0 + nrm((DEPTH, GLA_DV), 0.02)
    conv_w = nrm((DEPTH, SSD_CONV, SSD_CONV_DIM), SSD_CONV ** -0.5)
    conv_b = nrm((DEPTH, SSD_CONV_DIM), 0.02)
    dt0 = jnp.exp(jax.random.uniform(next(ks), (DEPTH, SSD_HEADS), f32,
                                     np.log(1e-3).astype(np.float32), np.log(1e-1).astype(np.float32)))
    dt_bias = dt0 + jnp.log(-jnp.expm1(-dt0))
    a_log = jnp.log(jax.random.uniform(next(ks), (DEPTH, SSD_HEADS), f32, 1.0, 16.0))
    d_skip = 1.0 + nrm((DEPTH, SSD_HEADS), 0.1)
    ssd_norm_w = 1.0 + nrm((DEPTH, SSD_WIDTH), 0.02)
    w_out = nrm((DEPTH, D_MIX, d), D_MIX ** -0.5)
    norm2_w = 1.0 + nrm((DEPTH, d), 0.02)
    ffn_w_gate = nrm((n_dense, d, D_FF), d ** -0.5)
    ffn_w_up = nrm((n_dense, d, D_FF), d ** -0.5)
    ffn_w_down = nrm((n_dense, D_FF, d), D_FF ** -0.5)
    router_w = nrm((n_moe, d, N_EXPERTS), d ** -0.5)
    moe_w_gate = nrm((n_moe, N_EXPERTS, d, D_FF), d ** -0.5)
    moe_w_up = nrm((n_moe, N_EXPERTS, d, D_FF), d ** -0.5)
    moe_w_down = nrm((n_moe, N_EXPERTS, D_FF, d), D_FF ** -0.5)
    final_norm_w = 1.0 + nrm((d,), 0.02)
    return {"x": x, "c": c, "ada_w": ada_w, "ada_b": ada_b, "norm1_w": norm1_w,
            "w_in": w_in, "gla_a_w": gla_a_w, "gla_a_b": gla_a_b, "gla_norm_w": gla_norm_w,
            "conv_w": conv_w, "conv_b": conv_b, "dt_bias": dt_bias, "a_log": a_log,
            "d_skip": d_skip, "ssd_norm_w": ssd_norm_w, "w_out": w_out, "norm2_w": norm2_w,
            "ffn_w_gate": ffn_w_gate, "ffn_w_up": ffn_w_up, "ffn_w_down": ffn_w_down,
            "router_w": router_w, "moe_w_gate": moe_w_gate, "moe_w_up": moe_w_up,
            "moe_w_down": moe_w_down, "final_norm_w": final_norm_w}


def reference(x, c, ada_w, ada_b, norm1_w, w_in, gla_a_w, gla_a_b, gla_norm_w,
              conv_w, conv_b, dt_bias, a_log, d_skip, ssd_norm_w, w_out, norm2_w,
              ffn_w_gate, ffn_w_up, ffn_w_down, router_w, moe_w_gate, moe_w_up,
              moe_w_down, final_norm_w):
    s = jax.nn.silu(c)
    for l in range(DEPTH):
        mod = s @ ada_w[l] + ada_b[l]
        sh1, sc1, g1, sh2, sc2, g2 = [m[:, None, :] for m in jnp.split(mod, 6, axis=-1)]
        h = rmsnorm(x, norm1_w[l]) * (1.0 + sc1) + sh1
        mix = hybrid_mixer(h, w_in[l], gla_a_w[l], gla_a_b[l], gla_norm_w[l], conv_w[l],
                           conv_b[l], dt_bias[l], a_log[l], d_skip[l], ssd_norm_w[l], w_out[l])
        x = x + g1 * mix
        h = rmsnorm(x, norm2_w[l]) * (1.0 + sc2) + sh2
        if l % 2 == 0:
            i = l // 2
            ff = swiglu(h, ffn_w_gate[i], ffn_w_up[i], ffn_w_down[i])
        else:
            i = l // 2
            ff = moe_swiglu(h, router_w[i], moe_w_gate[i], moe_w_up[i], moe_w_down[i])
        x = x + g2 * ff
    return rmsnorm(x, final_norm_w)
```

```python
import math
import numpy as np
from contextlib import ExitStack
import concourse.bass as bass
import concourse.mybir as mybir
from concourse.bass_utils import run_bass_kernel_spmd

F32 = mybir.dt.float32
BF16 = mybir.dt.bfloat16
AF = mybir.ActivationFunctionType
ALU = mybir.AluOpType
AX = mybir.AxisListType
EPS = 1e-6
NCORES = 8


class Buf:
    __slots__ = ("name", "w", "r")

    def __init__(self, name):
        self.name = name
        self.w = None
        self.r = []


class Sched:
    ENGS = ("pe", "act", "dve", "pool", "sp")

    def __init__(self, nc, stack):
        self.nc = nc
        self.stack = stack
        self.streams = {e: [] for e in self.ENGS}
        self.sem = {}
        self.cnt = {}
        for e in self.ENGS:
            self.sem[e] = stack.enter_context(nc.semaphore("s_" + e))
            self.cnt[e] = 0
        self.waited = {e: {} for e in self.ENGS}
        self.nsem = 0

    def new_sem(self, name):
        self.nsem += 1
        key = "d_%s_%d" % (name, self.nsem)
        self.sem[key] = self.stack.enter_context(self.nc.semaphore(key))
        self.cnt[key] = 0
        return key

    def op(self, eng, fn, reads=(), writes=(), dsem=None):
        deps = []
        for b in reads:
            deps.append(b.w)
        for b in writes:
            deps.append(b.w)
            deps.extend(b.r)
        w = self.waited[eng]
        best = {}
        for d in deps:
            if d is None:
                continue
            k, v = d
            if w.get(k, 0) >= v:
                continue
            if best.get(k, 0) < v:
                best[k] = v
        waits = list(best.items())
        for k, v in waits:
            w[k] = v
        if dsem is None:
            key, inc = eng, 1
        else:
            key, inc = dsem, 16
        self.cnt[key] += inc
        val = self.cnt[key]
        semh = self.sem[key]
        sems = self.sem

        def emit(e, waits=waits, fn=fn, semh=semh, inc=inc):
            for (k, v) in waits:
                e.wait_ge(sems[k], v)
            fn(e).then_inc(semh, inc)

        self.streams[eng].append(emit)
        tick = (key, val)
        for b in writes:
            b.w = tick
            b.r = []
        for b in reads:
            b.r.append(tick)

    def wait_all(self, eng, bufs):
        w = self.waited[eng]
        waits = []
        for b in bufs:
            for d in [b.w] + list(b.r):
                if d is None:
                    continue
                k, v = d
                if w.get(k, 0) >= v:
                    continue
                w[k] = v
                waits.append((k, v))
        sems = self.sem

        def emit(e, waits=waits):
            for (k, v) in waits:
                e.wait_ge(sems[k], v)

        self.streams[eng].append(emit)

    def emit_all(self):
        st = self.streams
        with self.nc.Block() as block:
            @block.tensor
            def _(e):
                for f in st["pe"]:
                    f(e)

            @block.scalar
            def _(e):
                for f in st["act"]:
                    f(e)

            @block.vector
            def _(e):
                for f in st["dve"]:
                    f(e)

            @block.gpsimd
            def _(e):
                for f in st["pool"]:
                    f(e)

            @block.sync
            def _(e):
                for f in st["sp"]:
                    f(e)


class K:
    def __init__(self, nc, S):
        self.nc, self.S = nc, S
        self.bufs = {}

    def B(self, name):
        if name not in self.bufs:
            self.bufs[name] = Buf(name)
        return self.bufs[name]

    def _b(self, names):
        return [self.B(n) for n in names]

    def sb(self, name, shape, dt=F32):
        return self.nc.alloc_sbuf_tensor("sb_" + name, shape, dt)

    def act(self, out, in_, func, r, w, **kw):
        self.S.op("act", lambda e: e.activation(out=out, in_=in_, func=func, **kw), self._b(r), self._b(w))

    def tt(self, out, in0, in1, op, r, w, eng="dve"):
        self.S.op(eng, lambda e: e.tensor_tensor(out=out, in0=in0, in1=in1, op=op), self._b(r), self._b(w))

    def ts(self, out, in0, s1, op0, r, w, s2=None, op1=None):
        if op1 is None:
            self.S.op("dve", lambda e: e.tensor_scalar(out=out, in0=in0, scalar1=s1, scalar2=None, op0=op0), self._b(r), self._b(w))
        else:
            self.S.op("dve", lambda e: e.tensor_scalar(out=out, in0=in0, scalar1=s1, scalar2=s2, op0=op0, op1=op1), self._b(r), self._b(w))

    def stt(self, out, in0, scalar, in1, op0, op1, r, w):
        self.S.op("dve", lambda e: e.scalar_tensor_tensor(out=out, in0=in0, scalar=scalar, in1=in1, op0=op0, op1=op1), self._b(r), self._b(w))

    def cp(self, out, in_, r, w, eng="dve"):
        self.S.op(eng, lambda e: e.tensor_copy(out=out, in_=in_), self._b(r), self._b(w))

    def ms(self, ap, val, w, eng="dve"):
        self.S.op(eng, lambda e: e.memset(ap, val), [], self._b(w))

    def rcp(self, out, in_, r, w):
        self.S.op("dve", lambda e: e.reciprocal(out=out, in_=in_), self._b(r), self._b(w))

    def red(self, out, in_, op, r, w):
        self.S.op("dve", lambda e: e.tensor_reduce(out=out, in_=in_, axis=AX.X, op=op), self._b(r), self._b(w))

    def mm(self, out, pairs, r, w):
        def f(e, out=out, pairs=pairs):
            ins = None
            n = len(pairs)
            for i, (l, rr) in enumerate(pairs):
                ins = e.matmul(out, lhsT=l, rhs=rr, start=(i == 0), stop=(i == n - 1))
            return ins
        self.S.op("pe", f, self._b(r), self._b(w))

    def tr(self, outs_ins, ident, r, w):
        def f(e, oi=outs_ins):
            ins = None
            for (o, i) in oi:
                ins = e.transpose(out=o, in_=i, identity=ident)
            return ins
        self.S.op("pe", f, self._b(r), self._b(w))

    def dma(self, eng, out, in_, r, w, sem=None):
        if sem is None:
            sem = self.S.new_sem("u")
        self.S.op(eng, lambda e: e.dma_start(out=out, in_=in_), self._b(r), self._b(w), dsem=sem)


def _consts():
    t = np.arange(128)
    T1 = (t[:, None] <= t[None, :]).astype(np.float32)
    T2 = (t[:, None] > t[None, :]).astype(np.float32)
    c = np.zeros((128, 8, 128), np.float32)
    c[:, 0] = np.eye(128, dtype=np.float32)
    c[:, 1] = T1
    c[:, 2] = T2
    c[:, 3] = T1 * (-1.0 / 16.0)
    c[:, 4] = T2 * (-1.0 / 16.0)
    c[:, 5] = 1.0
    c[:, 6] = T1
    c[:, 7] = np.where(T1 > 0, 0.0, -30000.0)
    return c


def _load_consts(nc, k, cst_ap, sem):
    c32 = k.sb("c32", [128, 8, 128])
    cbf = k.sb("cbf", [128, 8, 128], BF16)
    k.dma("sp", c32[:, :, :], cst_ap, [], ["c32"])
    k.cp(cbf[:, :, :], c32[:, :, :], ["c32"], ["cbf"])
    return c32, cbf


def _mod(nc, k, crep_ap, adaw_ap, adab_ap, ncols, banks, sem, wtmp, wtmp_buf):
    cr = k.sb("cr", [128, 8, 128])
    crb = k.sb("crb", [128, 8, 128], BF16)
    adabt = k.sb("adabt", [128, 512])
    mod = k.sb("mod", [128, ncols])
    k.dma("sp", cr[:, :, :], crep_ap, [], ["cr"])
    k.act(crb[:, :, :], cr[:, :, :], AF.Silu, ["cr"], ["crb"])
    wsem = k.S.new_sem("adaw")
    bsem = k.S.new_sem("adab")
    for n in range(ncols // 512):
        k.dma("pool", wtmp, adaw_ap[:, :, n * 512:(n + 1) * 512], [], [wtmp_buf], wsem)
        k.dma("sp", adabt[:, :], adab_ap[:, n * 512:(n + 1) * 512], [], ["adabt"], bsem)
        k.mm(banks[0][:, :], [(crb[:, kk, :], wtmp[:, kk, :]) for kk in range(8)], ["crb", wtmp_buf], ["bank0"])
        k.tt(mod[:, n * 512:(n + 1) * 512], banks[0][:, :], adabt[:, :], ALU.add, ["bank0", "adabt"], ["mod"])
    return mod


def _rmsnorm_stats(k, src, n, r, tag):
    junk, msq, rs = k.junk, k.msq, k.rs
    k.ms(msq[:, :], 0.0, ["msq"])
    k.act(junk[:, 0:n], src, AF.Square, r + ["msq"], ["junk", "msq"], scale=float(n) ** -0.5, accum_out=msq[:, 0:1])
    k.act(rs[:, :], msq[:, :], AF.Sqrt, ["msq"], ["rs"], bias=EPS, scale=1.0)
    k.rcp(rs[:, :], rs[:, :], ["rs"], ["rs"])
    return rs


def build_M():
    nc = bass.Bass("TRN2", target_bir_lowering=False)
    T = 8192
    NT = T // 128
    di = lambda n, s: nc.dram_tensor(n, s, F32, kind="ExternalInput").ap()
    x = di("x", [T, 1024]); crep = di("crep", [128, 8, 128]); adaw = di("adaw", [128, 8, 2048]); adab_d = di("adab", [128, 2048])
    n1w_d = di("n1w", [128, 1024]); wfm_d = di("wfm", [128, 8, 768]); wtm_d = di("wtm", [128, 8, 450])
    gaw_d = di("gaw", [128, 64]); gab_d = di("gab", [128, 64]); gnw_d = di("gnw", [128, 128])
    cw_d = di("cw", [128, 12]); cb_d = di("cb", [128, 3]); sm_d = di("sm", [128, 6]); cst_d = di("cst", [128, 8, 128])
    og = nc.dram_tensor("og", [T, 128], F32, kind="ExternalOutput").ap()
    ys = nc.dram_tensor("ys", [T, 128], F32, kind="ExternalOutput").ap()
    with ExitStack() as stack:
        S = Sched(nc, stack)
        k = K(nc, S)
        banks = [nc.alloc_psum_tensor("bank%d" % i, [128, 512], F32) for i in range(8)]
        ld = S.new_sem("ld")
        c32, cbf = _load_consts(nc, k, cst_d, ld)
        ident = cbf[:, 0, :]
        T1b, T2b, T1sb, T2sb, onesb = cbf[:, 1, :], cbf[:, 2, :], cbf[:, 3, :], cbf[:, 4, :], cbf[:, 5, :]
        T1f, onesf, mask01, negmask = c32[:, 1, :], c32[:, 5, :], c32[:, 6, :], c32[:, 7, :]
        k.junk = k.sb("junk", [128, 1024]); k.msq = k.sb("msq", [128, 1]); k.rs = k.sb("rs", [128, 1])
        wtmp = k.sb("wtmp", [128, 8, 512], BF16)
        mod = _mod(nc, k, crep, adaw, adab_d, 2048, banks, ld, wtmp[:, :, :], "wtmp")
        n1w = k.sb("n1w", [128, 1024]); A1 = k.sb("A1", [128, 1024])
        k.dma("sp", n1w[:, :], n1w_d, [], ["n1w"])
        k.stt(A1[:, :], mod[:, 1024:2048], 1.0, n1w[:, :], ALU.add, ALU.mult, ["mod", "n1w"], ["A1"])
        B1 = mod[:, 0:1024]
        wfm = k.sb("wfm", [128, 8, 768], BF16); wtm = k.sb("wtm", [128, 8, 450], BF16)
        wsem = S.new_sem("w")
        k.dma("pool", wfm[:, :, :], wfm_d, [], ["wfm"])
        k.dma("pool", wtm[:, :, :], wtm_d, [], ["wtm"])
        gaw32 = k.sb("gaw32", [128, 64]); gaw = k.sb("gawb", [128, 64], BF16); gab = k.sb("gab", [128, 64]); gnw = k.sb("gnw", [128, 128])
        cw = k.sb("cw", [128, 12]); cb = k.sb("cb", [128, 3]); sm = k.sb("sm", [128, 6]); aneg = k.sb("aneg", [128, 2])
        for (t_, d_, n_) in ((gaw32, gaw_d, "gaw32"), (gab, gab_d, "gab"), (gnw, gnw_d, "gnw"), (cw, cw_d, "cw"), (cb, cb_d, "cb"), (sm, sm_d, "sm")):
            k.dma("sp", t_[:, :], d_, [], [n_])
        k.cp(gaw[:, :], gaw32[:, :], ["gaw32"], ["gaw"])
        k.act(aneg[:, :], sm[:, 2:4], AF.Exp, ["sm"], ["aneg"])
        k.ts(aneg[:, :], aneg[:, :], -1.0, ALU.mult, ["aneg"], ["aneg"])
        dtb, dsk = sm[:, 0:2], sm[:, 4:6]
        xt = [k.sb("xt%d" % i, [128, 1024]) for i in range(2)]
        xsem = [S.new_sem("x0"), S.new_sem("x1")]
        tmpn = k.sb("tmpn", [128, 1024]); hbf = k.sb("hbf", [128, 1024], BF16); hT = k.sb("hT", [128, 8, 128], BF16)
        alrT = k.sb("alrT", [128, 128], BF16); xa = k.sb("xa", [128, 64]); e1 = k.sb("e1", [128, 64])
        sp = k.sb("sp", [128, 128]); sph = k.sb("sph", [128, 128], BF16); spl = k.sb("spl", [128, 128], BF16)
        E = k.sb("E", [128, 128]); Ei = k.sb("Ei", [128, 128]); Dc = k.sb("Dc", [128, 1])
        qeT = k.sb("qeT", [128, 128], BF16); keT = k.sb("keT", [128, 128], BF16)
        eb = k.sb("eb", [128, 64]); ktail = k.sb("ktail", [128, 128], BF16); vbf = k.sb("vbf", [128, 128], BF16)
        attb = k.sb("attb", [128, 128], BF16)
        S32 = k.sb("S32", [128, 128]); Sbf = k.sb("Sbf", [128, 128], BF16); tmpS = k.sb("tmpS", [128, 128])
        sg = k.sb("sg", [128, 128]); t1 = k.sb("t1", [128, 128]); ogs = k.sb("ogs", [128, 128])
        ub = k.sb("ub", [128, 3, 131]); acc = k.sb("acc", [128, 3, 128]); xc = k.sb("xc", [128, 3, 128], BF16)
        xstok = k.sb("xstok", [128, 128]); Btok = k.sb("Btok", [128, 128])
        dtr = k.sb("dtr", [128, 2]); e2 = k.sb("e2", [128, 2]); dt = k.sb("dt", [128, 2]); dtA = k.sb("dtA", [128, 2])
        dth = k.sb("dth", [128, 2], BF16); dtl = k.sb("dtl", [128, 2], BF16); hi32 = k.sb("hi32", [128, 2]); lo32 = k.sb("lo32", [128, 2])
        ecum = k.sb("ecum", [128, 2]); edec = k.sb("edec", [128, 2]); Dh = k.sb("Dh", [128, 2])
        Rh = k.sb("Rh", [128, 128], BF16); Rl = k.sb("Rl", [128, 128], BF16); Rbh = k.sb("Rbh", [128, 128], BF16); Rbl = k.sb("Rbl", [128, 128], BF16)
        seg = k.sb("seg", [128, 128]); decT = k.sb("decT", [128, 128]); W = k.sb("W", [128, 128], BF16)
        xdt = k.sb("xdt", [128, 2, 64], BF16); bdec = k.sb("bdec", [128, 128], BF16)
        ST32 = k.sb("ST32", [128, 2, 64]); STb = k.sb("STb", [128, 2, 64], BF16); tmpT = k.sb("tmpT", [128, 64])
        t2 = k.sb("t2", [128, 64]); t3 = k.sb("t3", [128, 64]); yy = k.sb("yy", [128, 128]); sz = k.sb("sz", [128, 128]); yss = k.sb("yss", [128, 128])
        osem = S.new_sem("o"); osem2 = S.new_sem("o2")
        for (ap_, n_) in ((sp[:, :], "sp"), (ktail[:, :], "ktail"), (S32[:, :], "S32"), (ub[:, :, :], "ub"), (ST32[:, :, :], "ST32")):
            k.ms(ap_, 0.0, [n_])
        k.cp(Sbf[:, :], S32[:, :], ["S32"], ["Sbf"])
        k.cp(STb[:, :, :], ST32[:, :, :], ["ST32"], ["STb"])
        b0v = banks[0][:, :].bitcast(BF16)
        fm = lambda c: (banks[1][:, c * 128:(c + 1) * 128] if c < 4 else banks[2][:, (c - 4) * 128:(c - 3) * 128])
        xa_ps = banks[2][:, 256:320]
        tm = banks[3]
        bT_ps = banks[4][:, 0:128]; blmb_ps = banks[4][:, 128:192]; cum_ps = banks[4][:, 192:194]; clmc_ps = banks[4][:, 194:196]; clast_ps = banks[4][:, 196:198]
        att_ps = banks[5][:, 0:128]; sc_ps = banks[5][:, 128:256]; seg_ps = banks[5][:, 256:384]
        o_ps = banks[6][:, 0:128]; ctr_ps = banks[6][:, 128:256]
        yin_ps = lambda h: banks[6][:, 256 + h * 64:320 + h * 64]
        yit_ps = lambda h: banks[6][:, 384 + h * 64:448 + h * 64]
        ctT_ps = lambda h: banks[7][:, h * 64:(h + 1) * 64]
        b7v = banks[7][:, :].bitcast(BF16)
        tp0, tp1 = b7v[:, 512:640], b7v[:, 640:768]
        LN8 = math.log(0.125)
        for t in range(NT):
            r0 = t * 128
            xb = "xt%d" % (t % 2)
            xtt = xt[t % 2]
            k.dma("sp", xtt[:, :], x[r0:r0 + 128, :], [], [xb], xsem[t % 2])
            rs = _rmsnorm_stats(k, xtt[:, :], 1024, [xb], "n1")
            k.stt(tmpn[:, :], xtt[:, :], rs[:, 0:1], A1[:, :], ALU.mult, ALU.mult, [xb, "rs", "A1"], ["tmpn"])
            k.tt(hbf[:, :], tmpn[:, :], B1, ALU.add, ["tmpn", "mod"], ["hbf"])
            k.tr([(b0v[:, c * 128:(c + 1) * 128], hbf[:, c * 128:(c + 1) * 128]) for c in range(8)], ident, ["hbf", "cbf"], ["bank0"])
            k.act(hT[:, :, :], b0v[:, :].rearrange("p (k n) -> p k n", k=8), AF.Copy, ["bank0"], ["hT"])
            for c in range(6):
                k.mm(fm(c), [(wfm[:, kk, c * 128:(c + 1) * 128], hT[:, kk, :]) for kk in range(8)], ["wfm", "hT"], ["fm%d" % c])
            k.mm(tm[:, 0:450], [(hT[:, kk, :], wtm[:, kk, :]) for kk in range(8)], ["wtm", "hT"], ["tm"])
            k.act(alrT[:, :], fm(5), AF.Copy, ["fm5"], ["alrT"])
            k.mm(xa_ps, [(alrT[:, :], gaw[:, :])], ["alrT", "gaw"], ["xa_ps"])
            k.tt(xa[:, :], xa_ps, gab[:, :], ALU.add, ["xa_ps", "gab"], ["xa"])
            k.act(e1[:, :], xa[:, :], AF.Exp, ["xa"], ["e1"], scale=-1.0)
            k.act(sp[:, 0:64], e1[:, :], AF.Ln, ["e1"], ["sp"], bias=1.0)
            k.cp(sph[:, :], sp[:, :], ["sp"], ["sph"])
            k.tt(spl[:, :], sp[:, :], sph[:, :], ALU.subtract, ["sp", "sph"], ["spl"])
            k.mm(bT_ps, [(sph[:, :], T1sb), (spl[:, :], T1sb)], ["sph", "spl", "cbf"], ["bT"])
            k.mm(blmb_ps, [(T2sb, sph[:, 0:64]), (T2sb, spl[:, 0:64])], ["sph", "spl", "cbf"], ["blmb"])
            k.act(E[:, :], bT_ps, AF.Exp, ["bT"], ["E"], bias=LN8)
            k.act(Ei[:, :], bT_ps, AF.Exp, ["bT"], ["Ei"], scale=-1.0)
            k.act(Dc[:, :], banks[4][:, 127:128], AF.Exp, ["bT"], ["Dc"])
            k.tt(qeT[:, :], fm(0), E[:, :], ALU.mult, ["fm0", "E"], ["qeT"])
            k.tt(keT[:, :], fm(1), Ei[:, :], ALU.mult, ["fm1", "Ei"], ["keT"])
            k.act(eb[:, :], blmb_ps, AF.Exp, ["blmb"], ["eb"])
            k.tt(ktail[:, 0:64], tm[:, 384:448], eb[:, :], ALU.mult, ["tm", "eb"], ["ktail"])
            k.act(vbf[:, :], tm[:, 0:128], AF.Copy, ["tm"], ["vbf"])
            k.mm(att_ps, [(keT[:, :], qeT[:, :])], ["keT", "qeT"], ["att"])
            k.tt(attb[:, :], att_ps, mask01, ALU.mult, ["att", "c32"], ["attb"])
            k.mm(o_ps, [(attb[:, :], vbf[:, :]), (qeT[:, :], Sbf[:, :])], ["attb", "vbf", "qeT", "Sbf"], ["o"])
            k.mm(ctr_ps, [(ktail[:, :], vbf[:, :])], ["ktail", "vbf"], ["ctr"])
            k.ts(tmpS[:, :], S32[:, :], Dc[:, 0:1], ALU.mult, ["S32", "Dc"], ["tmpS"])
            k.tt(S32[:, :], ctr_ps, tmpS[:, :], ALU.add, ["ctr", "tmpS"], ["S32"])
            k.cp(Sbf[:, :], S32[:, :], ["S32"], ["Sbf"])
            rs = _rmsnorm_stats(k, o_ps, 128, ["o"], "go")
            k.act(sg[:, :], tm[:, 128:256], AF.Silu, ["tm"], ["sg"])
            k.stt(t1[:, :], o_ps, rs[:, 0:1], gnw[:, :], ALU.mult, ALU.mult, ["o", "rs", "gnw"], ["t1"])
            k.tt(ogs[:, :], t1[:, :], sg[:, :], ALU.mult, ["t1", "sg"], ["ogs"])
            k.dma("sp", og[r0:r0 + 128, :], ogs[:, :], ["ogs"], ["out1"], osem)
            for blk in range(3):
                k.act(ub[:, blk, 3:131], fm(2 + blk), AF.Copy, ["fm%d" % (2 + blk)], ["ub"])
            for blk in range(3):
                k.act(acc[:, blk, :], ub[:, blk, 3:131], AF.Identity, ["ub", "cw", "cb"], ["acc"],
                      bias=cb[:, blk:blk + 1], scale=cw[:, blk * 4 + 3:blk * 4 + 4])
                for s in (1, 2, 3):
                    k.stt(acc[:, blk, :], ub[:, blk, 3 - s:131 - s], cw[:, blk * 4 + 3 - s:blk * 4 + 4 - s], acc[:, blk, :],
                          ALU.mult, ALU.add, ["ub", "cw", "acc"], ["acc"])
            k.act(xc[:, :, :], acc[:, :, :], AF.Silu, ["acc"], ["xc"])
            k.cp(ub[:, :, 0:3], ub[:, :, 128:131], ["ub"], ["ub"])
            k.tr([(tp0, xc[:, 0, :]), (tp1, xc[:, 1, :])], ident, ["xc", "cbf"], ["tp"])
            k.cp(xstok[:, :], tp0, ["tp"], ["xstok"])
            k.cp(Btok[:, :], tp1, ["tp"], ["Btok"])
            k.tt(dtr[:, :], tm[:, 448:450], dtb, ALU.add, ["tm", "sm"], ["dtr"])
            k.act(e2[:, :], dtr[:, :], AF.Exp, ["dtr"], ["e2"])
            k.act(dt[:, :], e2[:, :], AF.Ln, ["e2"], ["dt"], bias=1.0)
            k.tt(dtA[:, :], dt[:, :], aneg[:, :], ALU.mult, ["dt", "aneg"], ["dtA"])
            k.cp(dth[:, :], dtA[:, :], ["dtA"], ["dth"])
            k.cp(hi32[:, :], dth[:, :], ["dth"], ["hi32"])
            k.tt(lo32[:, :], dtA[:, :], hi32[:, :], ALU.subtract, ["dtA", "hi32"], ["lo32"])
            k.cp(dtl[:, :], lo32[:, :], ["lo32"], ["dtl"])
            k.mm(cum_ps, [(T1b, dth[:, :]), (T1b, dtl[:, :])], ["dth", "dtl", "cbf"], ["cum"])
            k.mm(clmc_ps, [(T2b, dth[:, :]), (T2b, dtl[:, :])], ["dth", "dtl", "cbf"], ["clmc"])
            k.mm(clast_ps, [(onesb, dth[:, :]), (onesb, dtl[:, :])], ["dth", "dtl", "cbf"], ["clast"])
            k.act(ecum[:, :], cum_ps, AF.Exp, ["cum"], ["ecum"])
            k.act(edec[:, :], clmc_ps, AF.Exp, ["clmc"], ["edec"])
            k.act(Dh[:, :], clast_ps, AF.Exp, ["clast"], ["Dh"])
            k.mm(sc_ps, [(xc[:, 1, :], xc[:, 2, :])], ["xc"], ["sc"])
            for h in range(2):
                hs = slice(h * 64, (h + 1) * 64)
                k.ts(Rh[:, :], T1f, hi32[:, h:h + 1], ALU.mult, ["c32", "hi32"], ["Rh"])
                k.ts(Rl[:, :], T1f, lo32[:, h:h + 1], ALU.mult, ["c32", "lo32"], ["Rl"])
                k.ts(Rbh[:, :], onesf, hi32[:, h:h + 1], ALU.mult, ["c32", "hi32"], ["Rbh"], s2=-1.0, op1=ALU.mult)
                k.ts(Rbl[:, :], onesf, lo32[:, h:h + 1], ALU.mult, ["c32", "lo32"], ["Rbl"], s2=-1.0, op1=ALU.mult)
                k.mm(seg_ps, [(onesb, Rh[:, :]), (onesb, Rl[:, :]), (T1b, Rbh[:, :]), (T1b, Rbl[:, :])], ["Rh", "Rl", "Rbh", "Rbl", "cbf"], ["segp"])
                k.tt(seg[:, :], seg_ps, negmask, ALU.min, ["segp", "c32"], ["seg"])
                k.act(decT[:, :], seg[:, :], AF.Exp, ["seg"], ["decT"])
                k.tt(W[:, :], sc_ps, decT[:, :], ALU.mult, ["sc", "decT"], ["W"])
                k.ts(xdt[:, h, :], xstok[:, hs], dt[:, h:h + 1], ALU.mult, ["xstok", "dt"], ["xdt"])
                k.ts(bdec[:, :], Btok[:, :], edec[:, h:h + 1], ALU.mult, ["Btok", "edec"], ["bdec"])
                k.mm(yin_ps(h), [(W[:, :], xdt[:, h, :])], ["W", "xdt"], ["yin"])
                k.mm(yit_ps(h), [(xc[:, 2, :], STb[:, h, :])], ["xc", "STb"], ["yit"])
                k.mm(ctT_ps(h), [(bdec[:, :], xdt[:, h, :])], ["bdec", "xdt"], ["ctT"])
                k.ts(t2[:, :], yit_ps(h), ecum[:, h:h + 1], ALU.mult, ["yit", "ecum"], ["t2"])
                k.tt(yy[:, hs], yin_ps(h), t2[:, :], ALU.add, ["yin", "t2"], ["yy"])
                k.ts(t3[:, :], xstok[:, hs], dsk[:, h:h + 1], ALU.mult, ["xstok", "sm"], ["t3"])
                k.tt(yy[:, hs], yy[:, hs], t3[:, :], ALU.add, ["yy", "t3"], ["yy"])
                k.ts(tmpT[:, :], ST32[:, h, :], Dh[:, h:h + 1], ALU.mult, ["ST32", "Dh"], ["tmpT"])
                k.tt(ST32[:, h, :], ctT_ps(h), tmpT[:, :], ALU.add, ["ctT", "tmpT"], ["ST32"])
                k.cp(STb[:, h, :], ST32[:, h, :], ["ST32"], ["STb"])
            k.act(sz[:, :], tm[:, 256:384], AF.Silu, ["tm"], ["sz"])
            k.tt(yss[:, :], yy[:, :], sz[:, :], ALU.mult, ["yy", "sz"], ["yss"])
            k.dma("sp", ys[r0:r0 + 128, :], yss[:, :], ["yss"], ["out2"], osem2)
        S.wait_all("sp", [k.B("out1"), k.B("out2")])
        S.emit_all()
    return nc


def build_F(n_exp, final):
    nc = bass.Bass("TRN2", target_bir_lowering=False)
    T = 2048
    NT = T // 128
    DFF = 3584
    NFB = DFF // 512
    di = lambda n, s: nc.dram_tensor(n, s, F32, kind="ExternalInput").ap()
    x = di("x", [T, 1024]); ogd = di("og", [T, 512]); ysd = di("ys", [T, 512])
    crep = di("crep", [128, 8, 128]); adaw = di("adaw", [128, 8, 4096]); adab_d = di("adab", [128, 4096])
    snw_d = di("snw", [128, 512]); wout_d = di("wout", [128, 8, 1024]); n2w_d = di("n2w", [128, 1024])
    wg_d = di("wg", [n_exp, 128, 8, DFF]); wu_d = di("wu", [n_exp, 128, 8, DFF]); wd_d = di("wd", [n_exp, 128, 28, 1024])
    fnw_d = di("fnw", [128, 1024]); cst_d = di("cst", [128, 8, 128])
    if n_exp > 1:
        rw_d = di("rw", [128, 8, 8])
    y = nc.dram_tensor("y", [T, 1024], F32, kind="ExternalOutput").ap()
    with ExitStack() as stack:
        S = Sched(nc, stack)
        k = K(nc, S)
        banks = [nc.alloc_psum_tensor("bank%d" % i, [128, 512], F32) for i in range(8)]
        ld = S.new_sem("ld")
        c32, cbf = _load_consts(nc, k, cst_d, ld)
        ident = cbf[:, 0, :]
        k.junk = k.sb("junk", [128, 1024]); k.msq = k.sb("msq", [128, 1]); k.rs = k.sb("rs", [128, 1])
        wAB = k.sb("wAB", [128, 2, 8, 512], BF16); wD = k.sb("wD", [128, 4, 1024], BF16)
        wA = wAB[:, 0, :, :]; wB = wAB[:, 1, :, :]
        wout = wAB[:, :, :, :].rearrange("p a k n -> p (a k n)").rearrange("p (k n) -> p k n", k=8)
        wDv = wD[:, :, :].rearrange("p a n -> p (a n)").rearrange("p (k n) -> p k n", k=8)
        mod = _mod(nc, k, crep, adaw, adab_d, 4096, banks, ld, wDv, "wD")
        g1, sh2, sc2, g2 = mod[:, 0:1024], mod[:, 1024:2048], mod[:, 2048:3072], mod[:, 3072:4096]
        n2w = k.sb("n2w", [128, 1024]); A2 = k.sb("A2", [128, 1024]); snw = k.sb("snw", [128, 512])
        k.dma("sp", n2w[:, :], n2w_d, [], ["n2w"])
        k.dma("sp", snw[:, :], snw_d, [], ["snw"])
        k.stt(A2[:, :], sc2, 1.0, n2w[:, :], ALU.add, ALU.mult, ["mod", "n2w"], ["A2"])
        semA = S.new_sem("wA"); semB = S.new_sem("wB"); semD = S.new_sem("wD")
        k.dma("pool", wout, wout_d, [], ["wA", "wB"], semA)
        x1 = k.sb("x1", [128, NT, 1024])
        h2T = k.sb("h2T", [128, 8, T], BF16)
        ogt = k.sb("ogt", [128, 512]); yst = k.sb("yst", [128, 512]); mrg = k.sb("mrg", [128, 1024], BF16); mT = k.sb("mT", [128, 8, 128], BF16)
        tmpn = k.sb("tmpn", [128, 1024]); h32 = k.sb("h32", [128, 1024]); hbf = k.sb("hbf", [128, 1024], BF16)
        tmph = k.sb("tmph", [128, 512])
        xsem = [S.new_sem("x%d" % i) for i in range(NT)]; ogsem = S.new_sem("og"); yssem = S.new_sem("ys")
        b0v = banks[0][:, :].bitcast(BF16)
        if n_exp > 1:
            rw32 = k.sb("rw32", [128, 8, 8]); rwh = k.sb("rwh", [128, 8, 8], BF16); rwh32 = k.sb("rwh32", [128, 8, 8]); rwl = k.sb("rwl", [128, 8, 8], BF16)
            lg = k.sb("lg", [128, 8]); gates = k.sb("gates", [128, NT, 8])
            hlo = k.sb("hlo", [128, 1024], BF16); hloT = k.sb("hloT", [128, 8, 128], BF16)
            m1 = k.sb("m1", [128, 1]); m2 = k.sb("m2", [128, 1]); mk1 = k.sb("mk1", [128, 8]); mk2 = k.sb("mk2", [128, 8]); l2 = k.sb("l2", [128, 8])
            dd = k.sb("dd", [128, 1]); ee = k.sb("ee", [128, 1]); p1 = k.sb("p1", [128, 1]); p2 = k.sb("p2", [128, 1]); gt = k.sb("gt", [128, 8])
            k.dma("sp", rw32[:, :, :], rw_d, [], ["rw32"])
            k.cp(rwh[:, :, :], rw32[:, :, :], ["rw32"], ["rwh"])
            k.cp(rwh32[:, :, :], rwh[:, :, :], ["rwh"], ["rwh32"])
            k.tt(rwl[:, :, :], rw32[:, :, :], rwh32[:, :, :], ALU.subtract, ["rw32", "rwh32"], ["rwl"])
            lg_ps = banks[7][:, 0:8]
        for t in range(NT):
            r0 = t * 128
            xb = "x1_%d" % t
            k.dma("sp", x1[:, t, :], x[r0:r0 + 128, :], [], [xb], xsem[t])
            k.dma("sp", ogt[:, :], ogd[r0:r0 + 128, :], [], ["ogt"], ogsem)
            k.dma("sp", yst[:, :], ysd[r0:r0 + 128, :], [], ["yst"], yssem)
            rs = _rmsnorm_stats(k, yst[:, :], 512, ["yst"], "sn")
            k.stt(tmph[:, :], yst[:, :], rs[:, 0:1], snw[:, :], ALU.mult, ALU.mult, ["yst", "rs", "snw"], ["tmph"])
            k.cp(mrg[:, 512:1024], tmph[:, :], ["tmph"], ["mrg"])
            k.cp(mrg[:, 0:512], ogt[:, :], ["ogt"], ["mrg"])
            k.tr([(b0v[:, c * 128:(c + 1) * 128], mrg[:, c * 128:(c + 1) * 128]) for c in range(8)], ident, ["mrg", "cbf"], ["bank0"])
            k.act(mT[:, :, :], b0v[:, :].rearrange("p (k n) -> p k n", k=8), AF.Copy, ["bank0"], ["mT"])
            for hf in range(2):
                k.mm(banks[1 + hf][:, :], [(mT[:, kk, :], wout[:, kk, hf * 512:(hf + 1) * 512]) for kk in range(8)], ["mT", "wA", "wB"], ["bk%d" % (1 + hf)])
                k.tt(tmph[:, :], banks[1 + hf][:, :], g1[:, hf * 512:(hf + 1) * 512], ALU.mult, ["bk%d" % (1 + hf), "mod"], ["tmph"])
                k.tt(x1[:, t, hf * 512:(hf + 1) * 512], x1[:, t, hf * 512:(hf + 1) * 512], tmph[:, :], ALU.add, [xb, "tmph"], [xb])
            rs = _rmsnorm_stats(k, x1[:, t, :], 1024, [xb], "n2")
            k.stt(tmpn[:, :], x1[:, t, :], rs[:, 0:1], A2[:, :], ALU.mult, ALU.mult, [xb, "rs", "A2"], ["tmpn"])
            k.tt(h32[:, :], tmpn[:, :], sh2, ALU.add, ["tmpn", "mod"], ["h32"])
            k.cp(hbf[:, :], h32[:, :], ["h32"], ["hbf"])
            k.tr([(b0v[:, c * 128:(c + 1) * 128], hbf[:, c * 128:(c + 1) * 128]) for c in range(8)], ident, ["hbf", "cbf"], ["bank0"])
            k.act(h2T[:, :, r0:r0 + 128], b0v[:, :].rearrange("p (k n) -> p k n", k=8), AF.Copy, ["bank0"], ["h2T"])
            if n_exp > 1:
                k.tt(hlo[:, :], h32[:, :], hbf[:, :], ALU.subtract, ["h32", "hbf"], ["hlo"])
                k.tr([(b0v[:, c * 128:(c + 1) * 128], hlo[:, c * 128:(c + 1) * 128]) for c in range(8)], ident, ["hlo", "cbf"], ["bank0"])
                k.act(hloT[:, :, :], b0v[:, :].rearrange("p (k n) -> p k n", k=8), AF.Copy, ["bank0"], ["hloT"])
                k.mm(lg_ps, [(h2T[:, kk, r0:r0 + 128], rwh[:, kk, :]) for kk in range(8)] + [(h2T[:, kk, r0:r0 + 128], rwl[:, kk, :]) for kk in range(8)]
                     + [(hloT[:, kk, :], rwh[:, kk, :]) for kk in range(8)], ["h2T", "hloT", "rwh", "rwl"], ["lgps"])
                k.cp(lg[:, :], lg_ps, ["lgps"], ["lg"])
                k.red(m1[:, :], lg[:, :], ALU.max, ["lg"], ["m1"])
                k.ts(mk1[:, :], lg[:, :], m1[:, 0:1], ALU.is_equal, ["lg", "m1"], ["mk1"])
                k.stt(l2[:, :], mk1[:, :], -1e30, lg[:, :], ALU.mult, ALU.add, ["mk1", "lg"], ["l2"])
                k.red(m2[:, :], l2[:, :], ALU.max, ["l2"], ["m2"])
                k.ts(mk2[:, :], l2[:, :], m2[:, 0:1], ALU.is_equal, ["l2", "m2"], ["mk2"])
                k.tt(dd[:, :], m2[:, :], m1[:, :], ALU.subtract, ["m1", "m2"], ["dd"])
                k.act(ee[:, :], dd[:, :], AF.Exp, ["dd"], ["ee"])
                k.ts(p1[:, :], ee[:, :], 1.0, ALU.add, ["ee"], ["p1"])
                k.rcp(p1[:, :], p1[:, :], ["p1"], ["p1"])
                k.tt(p2[:, :], ee[:, :], p1[:, :], ALU.mult, ["ee", "p1"], ["p2"])
                k.ts(gt[:, :], mk1[:, :], p1[:, 0:1], ALU.mult, ["mk1", "p1"], ["gt"])
                k.stt(gates[:, t, :], mk2[:, :], p2[:, 0:1], gt[:, :], ALU.mult, ALU.add, ["mk2", "p2", "gt"], ["gates"])
        sgt = k.sb("sgt", [128, 512]); hid = k.sb("hid", [128, 4, 512], BF16); ev = k.sb("ev", [128, 512])
        xall = ["x1_%d" % t for t in range(NT)]
        for e_ in range(n_exp):
            for fb in range(NFB):
                k.dma("pool", wA, wg_d[e_, :, :, fb * 512:(fb + 1) * 512], [], ["wA"], semA)
                k.dma("pool", wB, wu_d[e_, :, :, fb * 512:(fb + 1) * 512], [], ["wB"], semB)
                k.dma("pool", wD[:, :, :], wd_d[e_, :, fb * 4:(fb + 1) * 4, :], [], ["wD"], semD)
                for tb in range(T // 512):
                    ts_ = slice(tb * 512, (tb + 1) * 512)
                    for sub in range(4):
                        k.mm(banks[3][:, :], [(wA[:, kk, sub * 128:(sub + 1) * 128], h2T[:, kk, ts_]) for kk in range(8)], ["wA", "h2T"], ["bk3"])
                        k.mm(banks[4][:, :], [(wB[:, kk, sub * 128:(sub + 1) * 128], h2T[:, kk, ts_]) for kk in range(8)], ["wB", "h2T"], ["bk4"])
                        k.act(sgt[:, :], banks[3][:, :], AF.Silu, ["bk3"], ["sgt"])
                        k.tt(hid[:, sub, :], banks[4][:, :], sgt[:, :], ALU.mult, ["bk4", "sgt"], ["hid"])
                    for ti in range(4):
                        t = tb * 4 + ti
                        xb = "x1_%d" % t
                        for hf in range(2):
                            bk = 5 + hf
                            k.mm(banks[bk][:, :], [(hid[:, sub, ti * 128:(ti + 1) * 128], wD[:, sub, hf * 512:(hf + 1) * 512]) for sub in range(4)],
                                 ["hid", "wD"], ["bk%d" % bk])
                            k.tt(ev[:, :], banks[bk][:, :], g2[:, hf * 512:(hf + 1) * 512], ALU.mult, ["bk%d" % bk, "mod"], ["ev"])
                            xs_ = x1[:, t, hf * 512:(hf + 1) * 512]
                            if n_exp > 1:
                                k.stt(xs_, ev[:, :], gates[:, t, e_:e_ + 1], xs_, ALU.mult, ALU.add, ["ev", "gates", xb], [xb])
                            else:
                                k.tt(xs_, xs_, ev[:, :], ALU.add, ["ev", xb], [xb])
        osem = S.new_sem("o")
        if final:
            fnw = k.sb("fnw", [128, 1024])
            k.dma("sp", fnw[:, :], fnw_d, [], ["fnw"])
        for t in range(NT):
            r0 = t * 128
            xb = "x1_%d" % t
            if final:
                rs = _rmsnorm_stats(k, x1[:, t, :], 1024, [xb], "fn")
                k.stt(x1[:, t, :], x1[:, t, :], rs[:, 0:1], fnw[:, :], ALU.mult, ALU.mult, [xb, "rs", "fnw"], [xb])
            k.dma("sp", y[r0:r0 + 128, :], x1[:, t, :], [xb], ["out"], osem)
        S.wait_all("sp", [k.B("out")])
        S.emit_all()
    return nc


_PROGS = {}


def _prog(key):
    if key not in _PROGS:
        if key == "M":
            _PROGS[key] = build_M()
        else:
            _PROGS[key] = build_F(key[1], key[2])
    return _PROGS[key]


def _pk(w):
    n = w.shape[1]
    return np.ascontiguousarray(w.reshape(8, 128, n).transpose(1, 0, 2))


def _rep(v, n=128):
    return np.ascontiguousarray(np.broadcast_to(np.asarray(v, np.float32).reshape(1, -1), (n, v.size)))


def _pad_cols(w, width=128):
    out = np.zeros((w.shape[0], width), np.float32)
    out[:, :w.shape[1]] = w
    return out


def kernel(x, c, ada_w, ada_b, norm1_w, w_in, gla_a_w, gla_a_b, gla_norm_w, conv_w, conv_b, dt_bias, a_log,
           d_skip, ssd_norm_w, w_out, norm2_w, ffn_w_gate, ffn_w_up, ffn_w_down, router_w, moe_w_gate,
           moe_w_up, moe_w_down, final_norm_w):
    f = lambda a: np.asarray(a, np.float32)
    x = f(x); c = f(c)
    cst = _consts()
    xcur = x.copy()
    creps = [np.ascontiguousarray(np.broadcast_to(c[b].reshape(8, 128).T[:, :, None], (128, 8, 128))) for b in range(2)]
    OQ, OK_, OV, OG, OA, OZ, OX, OB, OC, ODT = 0, 256, 512, 1024, 1536, 1552, 2064, 2576, 2704, 2832
    for l in range(4):
        wi = f(w_in[l])
        adawl = f(ada_w[l]); adabl = f(ada_b[l])
        in_maps = []
        for core in range(NCORES):
            b, j = core // 4, core % 4
            g = j // 2
            fmcols = np.concatenate([
                _pad_cols(wi[:, OQ + 64 * j:OQ + 64 * j + 64]), _pad_cols(wi[:, OK_ + 64 * j:OK_ + 64 * j + 64]),
                wi[:, OX + 128 * j:OX + 128 * j + 128], _pad_cols(wi[:, OB + 64 * g:OB + 64 * g + 64]),
                _pad_cols(wi[:, OC + 64 * g:OC + 64 * g + 64]), _pad_cols(wi[:, OA:OA + 16])], axis=1)
            tmcols = np.concatenate([wi[:, OV + 128 * j:OV + 128 * j + 128], wi[:, OG + 128 * j:OG + 128 * j + 128],
                                     wi[:, OZ + 128 * j:OZ + 128 * j + 128], wi[:, OK_ + 64 * j:OK_ + 64 * j + 64],
                                     wi[:, ODT + 2 * j:ODT + 2 * j + 2]], axis=1)
            gaw = np.zeros((128, 64), np.float32); gaw[:16] = f(gla_a_w[l])[:, 64 * j:64 * j + 64]
            cwl = f(conv_w[l]); cbl = f(conv_b[l])
            cw = np.zeros((128, 12), np.float32); cb = np.zeros((128, 3), np.float32)
            cw[:, 0:4] = cwl[:, 128 * j:128 * j + 128].T; cb[:, 0] = cbl[128 * j:128 * j + 128]
            cw[:64, 4:8] = cwl[:, 512 + 64 * g:512 + 64 * g + 64].T; cb[:64, 1] = cbl[512 + 64 * g:512 + 64 * g + 64]
            cw[:64, 8:12] = cwl[:, 640 + 64 * g:640 + 64 * g + 64].T; cb[:64, 2] = cbl[640 + 64 * g:640 + 64 * g + 64]
            sm = np.concatenate([f(dt_bias[l])[2 * j:2 * j + 2], f(a_log[l])[2 * j:2 * j + 2], f(d_skip[l])[2 * j:2 * j + 2]])
            in_maps.append({
                "x": np.ascontiguousarray(xcur[b]), "crep": creps[b], "adaw": _pk(adawl[:, 0:2048]), "adab": _rep(adabl[0:2048]),
                "n1w": _rep(f(norm1_w[l])), "wfm": _pk(fmcols), "wtm": _pk(tmcols), "gaw": gaw,
                "gab": _rep(f(gla_a_b[l])[64 * j:64 * j + 64]), "gnw": _rep(f(gla_norm_w[l])), "cw": cw, "cb": cb,
                "sm": _rep(sm), "cst": cst})
        res = run_bass_kernel_spmd(_prog("M"), in_maps, core_ids=list(range(NCORES)))
        og = np.stack([np.concatenate([res.results[b * 4 + j]["og"] for j in range(4)], axis=1) for b in range(2)])
        ysd = np.stack([np.concatenate([res.results[b * 4 + j]["ys"] for j in range(4)], axis=1) for b in range(2)])
        moe = (l % 2 == 1)
        i = l // 2
        if moe:
            wg, wu, wd = f(moe_w_gate[i]), f(moe_w_up[i]), f(moe_w_down[i])
        else:
            wg, wu, wd = f(ffn_w_gate[i])[None], f(ffn_w_up[i])[None], f(ffn_w_down[i])[None]
        ne = wg.shape[0]
        wgp = np.ascontiguousarray(wg.reshape(ne, 8, 128, 3584).transpose(0, 2, 1, 3))
        wup = np.ascontiguousarray(wu.reshape(ne, 8, 128, 3584).transpose(0, 2, 1, 3))
        wdp = np.ascontiguousarray(wd.reshape(ne, 28, 128, 1024).transpose(0, 2, 1, 3))
        common = {"adaw": _pk(adawl[:, 2048:6144]), "adab": _rep(adabl[2048:6144]), "snw": _rep(f(ssd_norm_w[l])),
                  "wout": _pk(f(w_out[l])), "n2w": _rep(f(norm2_w[l])), "wg": wgp, "wu": wup, "wd": wdp,
                  "fnw": _rep(f(final_norm_w)), "cst": cst}
        if moe:
            common["rw"] = _pk(f(router_w[i]))
        in_maps = []
        for core in range(NCORES):
            b, s = core // 4, core % 4
            sl = slice(s * 2048, (s + 1) * 2048)
            m = dict(common)
            m.update({"x": np.ascontiguousarray(xcur[b, sl]), "og": np.ascontiguousarray(og[b, sl]),
                      "ys": np.ascontiguousarray(ysd[b, sl]), "crep": creps[b]})
            in_maps.append(m)
        res = run_bass_kernel_spmd(_prog(("F", ne, l == 3)), in_maps, core_ids=list(range(NCORES)))
        xcur = np.stack([np.concatenate([res.results[b * 4 + s]["y"] for s in range(4)], axis=0) for b in range(2)])
    return xcur.astype(np.float32)
```
